# Optimizing a Trainium2 kernel written in Bass

```python
import math
import jax
import jax.numpy as jnp
from jax import lax
import numpy as np

D_MODEL = 2048
BATCH = 4
SEQ = 4096
DEPTH = 4

GRID_W = 64
CTX_LEN = 256

N_HEADS = 8
N_KV_HEADS = 2
HEAD_DIM = 128
Q_GROUP = N_HEADS // N_KV_HEADS
ATTN_WIDTH = N_HEADS * HEAD_DIM
KV_WIDTH = N_KV_HEADS * HEAD_DIM
WINDOW = 128
ATTN_BLOCK = 128
ROPE_THETA = 10000.0
MASK_VALUE = -1e30

HYENA_WIDTH = 512
HYENA_SHORT = 3
HYENA_BANDS = 16
HYENA_EMB = 1 + 2 * HYENA_BANDS
HYENA_FILTER_HIDDEN = 64
HYENA_TARGET = 1e-2
HYENA_DECAY_PCT_HI = 0.3
HYENA_DECAY_PCT_LO = 1.5
HYENA_MAX_DECAY = math.log(HYENA_TARGET) / HYENA_DECAY_PCT_HI
HYENA_MIN_DECAY = math.log(HYENA_TARGET) / HYENA_DECAY_PCT_LO

FNET_WIDTH = 512
FNET_GROUPS = 4
FNET_GROUP_DIM = FNET_WIDTH // FNET_GROUPS

N_BRANCHES = 3

Q_OFF = 0
K_OFF = Q_OFF + ATTN_WIDTH
V_OFF = K_OFF + KV_WIDTH
HY_OFF = V_OFF + KV_WIDTH
FN_OFF = HY_OFF + 3 * HYENA_WIDTH
GATE_OFF = FN_OFF + FNET_WIDTH
IN_WIDTH = GATE_OFF + N_BRANCHES * D_MODEL

N_GROUPS = 4
EXPERTS_PER_GROUP = 8
N_EXPERTS = N_GROUPS * EXPERTS_PER_GROUP
TOP_K = 2
EXPERT_HIDDEN = 512
MOE_BLOCK = 128

LN_EPS = 1e-6
DEEPNORM_ALPHA = (2 * DEPTH) ** 0.25
DEEPNORM_BETA = (8 * DEPTH) ** -0.25

kernel_name = "hybrid_hyena_fnet_swa_hmoe_dit_trunk"


def layer_norm(x):
    xf = x.astype(jnp.float32)
    mu = jnp.mean(xf, -1, keepdims=True)
    var = jnp.mean(jnp.square(xf - mu), -1, keepdims=True)
    return ((xf - mu) * lax.rsqrt(var + LN_EPS)).astype(x.dtype)


def modulate(x, shift, scale):
    return layer_norm(x) * (1 + scale) + shift


def post_ln(x, g, b):
    return layer_norm(x) * g + b


def axial_rope_angles(seq_len):
    rows = seq_len // GRID_W
    row = jnp.broadcast_to(jnp.arange(rows)[:, None], (rows, GRID_W)).reshape(-1)
    col = jnp.broadcast_to(jnp.arange(GRID_W)[None, :], (rows, GRID_W)).reshape(-1)
    n_freq = HEAD_DIM // 4
    inv_freq = ROPE_THETA ** (-jnp.arange(n_freq, dtype=jnp.float32) / n_freq)
    ang_row = row.astype(jnp.float32)[:, None] * inv_freq[None, :]
    ang_col = col.astype(jnp.float32)[:, None] * inv_freq[None, :]
    return ang_row, ang_col


def _rotate(x, ang):
    n = ang.shape[-1]
    cos = jnp.cos(ang)[None, :, None, :]
    sin = jnp.sin(ang)[None, :, None, :]
    x1, x2 = x[..., :n], x[..., n:]
    return jnp.concatenate([x1 * cos - x2 * sin, x2 * cos + x1 * sin], -1)


def apply_axial_rope(x, ang_row, ang_col):
    half = HEAD_DIM // 2
    xf = x.astype(jnp.float32)
    out = jnp.concatenate([_rotate(xf[..., :half], ang_row), _rotate(xf[..., half:], ang_col)], -1)
    return out.astype(x.dtype)


def sink_probs(logits, sink):
    m = jnp.maximum(jnp.max(logits, -1, keepdims=True), sink)
    e = jnp.exp(logits - m)
    return e / (jnp.sum(e, -1, keepdims=True) + jnp.exp(sink - m))


def latent_window_attention(q, k, v, kc, vc, sink):
    b_, s_, _, _ = q.shape
    nb = s_ // ATTN_BLOCK
    scale = HEAD_DIM ** -0.5
    qb = q.reshape(b_, nb, ATTN_BLOCK, N_KV_HEADS, Q_GROUP, HEAD_DIM)

    def windows(t):
        tb = t.reshape(b_, nb, ATTN_BLOCK, N_KV_HEADS, HEAD_DIM)
        tp = jnp.pad(tb, ((0, 0), (1, 1), (0, 0), (0, 0), (0, 0)))
        return jnp.concatenate([tp[:, :-2], tp[:, 1:-1], tp[:, 2:]], axis=2)

    kw, vw = windows(k), windows(v)
    s_loc = jnp.einsum('bnqhgd,bnjhd->bnhgqj', qb, kw, preferred_element_type=jnp.float32) * scale
    s_ctx = jnp.einsum('bnqhgd,bchd->bnhgqc', qb, kc, preferred_element_type=jnp.float32) * scale
    blk = jnp.arange(nb)[:, None, None]
    qpos = blk * ATTN_BLOCK + jnp.arange(ATTN_BLOCK)[None, :, None]
    kpos = (blk - 1) * ATTN_BLOCK + jnp.arange(3 * ATTN_BLOCK)[None, None, :]
    valid = (jnp.abs(qpos - kpos) <= WINDOW) & (kpos >= 0) & (kpos < s_)
    s_loc = jnp.where(valid[None, :, None, None], s_loc, MASK_VALUE)
    sink_b = sink.astype(jnp.float32).reshape(1, 1, N_KV_HEADS, Q_GROUP, 1, 1)
    p = sink_probs(jnp.concatenate([s_loc, s_ctx], -1), sink_b)
    p_loc = p[..., :3 * ATTN_BLOCK].astype(v.dtype)
    p_ctx = p[..., 3 * ATTN_BLOCK:].astype(vc.dtype)
    o = (jnp.einsum('bnhgqj,bnjhd->bnqhgd', p_loc, vw)
         + jnp.einsum('bnhgqc,bchd->bnqhgd', p_ctx, vc))
    return o.reshape(b_, s_, ATTN_WIDTH)


def context_attention(qc, kc, vc, sink):
    b_, c_, _, _ = qc.shape
    scale = HEAD_DIM ** -0.5
    qg = qc.reshape(b_, c_, N_KV_HEADS, Q_GROUP, HEAD_DIM)
    s = jnp.einsum('bqhgd,bchd->bhgqc', qg, kc, preferred_element_type=jnp.float32) * scale
    p = sink_probs(s, sink.astype(jnp.float32).reshape(1, N_KV_HEADS, Q_GROUP, 1, 1))
    o = jnp.einsum('bhgqc,bchd->bqhgd', p.astype(vc.dtype), vc)
    return o.reshape(b_, c_, ATTN_WIDTH)


def short_conv(u, w, b):
    n = u.shape[1]
    pad = HYENA_SHORT // 2
    up = jnp.pad(u, ((0, 0), (pad, pad), (0, 0)))
    out = b
    for j in range(HYENA_SHORT):
        out = out + up[:, j:j + n] * w[j]
    return out


def hyena_filter(n, w1, b1, w2, b2, w3, b3, w4, b4, freq):
    f32 = jnp.float32
    t = jnp.linspace(0.0, 1.0, n, dtype=f32)[:, None]
    w = 2.0 * math.pi * jnp.arange(n, dtype=f32)[:, None] / n
    bands = jnp.linspace(1e-4, HYENA_BANDS - 1, HYENA_BANDS, dtype=f32)[None, :]
    z = jnp.concatenate([t, jnp.cos(bands * w), -jnp.sin(bands * w)], -1)
    fr = freq.astype(f32)
    hdn = jnp.sin(fr[0] * (z @ w1.astype(f32) + b1.astype(f32)))
    hdn = jnp.sin(fr[1] * (hdn @ w2.astype(f32) + b2.astype(f32)))
    hdn = jnp.sin(fr[2] * (hdn @ w3.astype(f32) + b3.astype(f32)))
    filt = (hdn @ w4.astype(f32) + b4.astype(f32)).reshape(n, 2, HYENA_WIDTH)
    deltas = jnp.abs(jnp.linspace(HYENA_MIN_DECAY, HYENA_MAX_DECAY, HYENA_WIDTH, dtype=f32))
    filt = filt * jnp.exp(-t * deltas[None, :])[:, None, :]
    circ = jnp.concatenate([filt[:, 0], jnp.zeros((1, HYENA_WIDTH), f32), filt[:0:-1, 1]], 0)
    return circ / (jnp.sum(jnp.abs(circ), 0, keepdims=True) + 1e-6)


def hyena_branch(u, conv_w, conv_b, filt_params, d_bias):
    n = u.shape[1]
    uc = short_conv(u, conv_w, conv_b)
    x0, x1, v = jnp.split(uc, 3, axis=-1)
    circ = hyena_filter(n, *filt_params)
    z = (x1 * v).astype(jnp.float32)
    zf = jnp.fft.rfft(z, n=2 * n, axis=1)
    hf = jnp.fft.rfft(circ, n=2 * n, axis=0)
    y = jnp.fft.irfft(zf * hf[None], n=2 * n, axis=1)[:, :n] + z * d_bias.astype(jnp.float32)
    return x0 * y.astype(u.dtype)


def fnet_branch(u):
    b_, n, _ = u.shape
    ug = u.astype(jnp.float32).reshape(b_, n, FNET_GROUPS, FNET_GROUP_DIM)
    y = jnp.fft.fftn(ug, axes=(1, 3), norm='ortho').real
    return y.reshape(b_, n, FNET_WIDTH).astype(u.dtype)


def mixer_output(p, y_attn, conv_w, conv_b, filt_params, d_bias, w_ba, w_bh, w_bf, w_o):
    y_hy = hyena_branch(p[..., HY_OFF:FN_OFF], conv_w, conv_b, filt_params, d_bias)
    y_fn = fnet_branch(p[..., FN_OFF:GATE_OFF])
    g = jax.nn.sigmoid(p[..., GATE_OFF:].astype(jnp.float32)).astype(p.dtype)
    g_a, g_h, g_f = jnp.split(g, N_BRANCHES, axis=-1)
    y = g_a * (y_attn @ w_ba) + g_h * (y_hy @ w_bh) + g_f * (y_fn @ w_bf)
    return y @ w_o


def hier_route(h, wg, bg, we, be):
    n_tok = h.shape[0]
    g_logits = (h @ wg + bg).astype(jnp.float32)
    g_prob = jax.nn.softmax(g_logits, -1)
    grp = jnp.argmax(g_logits, -1)
    rows = jnp.arange(n_tok)
    g_w = g_prob[rows, grp][:, None]
    e_logits = (h @ we + be).astype(jnp.float32).reshape(n_tok, N_GROUPS, EXPERTS_PER_GROUP)
    e_in = e_logits[rows, grp]
    top_v, top_i = lax.top_k(e_in, TOP_K)
    weights = jax.nn.softmax(top_v, -1) * g_w
    expert_id = grp[:, None] * EXPERTS_PER_GROUP + top_i
    return expert_id, weights


def moe_forward(h, expert_id, weights, w1, w3, w2):
    n_tok, d = h.shape
    n_asg = n_tok * TOP_K
    e_flat = expert_id.reshape(-1)
    tok = jnp.repeat(jnp.arange(n_tok, dtype=jnp.int32), TOP_K)
    w_flat = weights.reshape(-1)
    order = jnp.argsort(e_flat)
    e_s, tok_s, w_s = e_flat[order], tok[order], w_flat[order]
    counts = jnp.zeros((N_EXPERTS,), jnp.int32).at[e_flat].add(1)
    padded = ((counts + MOE_BLOCK - 1) // MOE_BLOCK) * MOE_BLOCK
    start = jnp.cumsum(counts) - counts
    pend = jnp.cumsum(padded)
    pstart = pend - padded
    dest = pstart[e_s] + (jnp.arange(n_asg, dtype=jnp.int32) - start[e_s])
    n_blocks = -(-(n_asg + N_EXPERTS * (MOE_BLOCK - 1)) // MOE_BLOCK)
    n_slots = n_blocks * MOE_BLOCK
    slot_tok = jnp.full((n_slots,), n_tok, jnp.int32).at[dest].set(tok_s)
    slot_w = jnp.zeros((n_slots,), h.dtype).at[dest].set(w_s.astype(h.dtype))
    block_e = jnp.minimum(jnp.searchsorted(pend, jnp.arange(n_blocks, dtype=jnp.int32) * MOE_BLOCK, side='right'), N_EXPERTS - 1)
    h_pad = jnp.concatenate([h, jnp.zeros((1, d), h.dtype)], 0)
    xb = h_pad[slot_tok].reshape(n_blocks, MOE_BLOCK, d)

    def expert_block(args):
        xblk, e = args
        return (jax.nn.silu(xblk @ w1[e]) * (xblk @ w3[e])) @ w2[e]

    yb = lax.map(expert_block, (xb, block_e))
    out = jnp.zeros((n_tok + 1, d), h.dtype).at[slot_tok].add(yb.reshape(n_slots, d) * slot_w[:, None])
    return out[:n_tok]


def setup_inputs(seed: int = 0) -> dict:
    key = jax.random.key(seed)
    ks = iter(jax.random.split(key, 48))

    def nrm(shape, scale):
        return jax.random.normal(next(ks), shape, jnp.float32) * scale

    d = D_MODEL
    return {
        'x': nrm((BATCH, SEQ, d), 1.0),
        'c': nrm((BATCH, d), 1.0),
        'ctx': nrm((BATCH, CTX_LEN, d), 1.0),
        'c_ctx': nrm((d,), 1.0),
        'w_ada': nrm((DEPTH, d, 6 * d), 0.5 * d ** -0.5),
        'b_ada': nrm((DEPTH, 6 * d), 0.01),
        'w_in': nrm((DEPTH, d, IN_WIDTH), d ** -0.5),
        'conv_w': nrm((DEPTH, HYENA_SHORT, 3 * HYENA_WIDTH), HYENA_SHORT ** -0.5),
        'conv_b': nrm((DEPTH, 3 * HYENA_WIDTH), 0.01),
        'hy_w1': nrm((DEPTH, HYENA_EMB, HYENA_FILTER_HIDDEN), HYENA_EMB ** -0.5),
        'hy_b1': nrm((DEPTH, HYENA_FILTER_HIDDEN), 0.01),
        'hy_w2': nrm((DEPTH, HYENA_FILTER_HIDDEN, HYENA_FILTER_HIDDEN), HYENA_FILTER_HIDDEN ** -0.5),
        'hy_b2': nrm((DEPTH, HYENA_FILTER_HIDDEN), 0.01),
        'hy_w3': nrm((DEPTH, HYENA_FILTER_HIDDEN, HYENA_FILTER_HIDDEN), HYENA_FILTER_HIDDEN ** -0.5),
        'hy_b3': nrm((DEPTH, HYENA_FILTER_HIDDEN), 0.01),
        'hy_w4': nrm((DEPTH, HYENA_FILTER_HIDDEN, 2 * HYENA_WIDTH), HYENA_FILTER_HIDDEN ** -0.5),
        'hy_b4': nrm((DEPTH, 2 * HYENA_WIDTH), 0.01),
        'hy_freq': 1.0 + nrm((DEPTH, 3, HYENA_FILTER_HIDDEN), 0.01),
        'hy_dbias': nrm((DEPTH, HYENA_WIDTH), 1.0),
        'attn_sink': nrm((DEPTH, N_HEADS), 1.0),
        'w_br_attn': nrm((DEPTH, ATTN_WIDTH, d), ATTN_WIDTH ** -0.5 * DEEPNORM_BETA),
        'w_br_hyena': nrm((DEPTH, HYENA_WIDTH, d), HYENA_WIDTH ** -0.5 * DEEPNORM_BETA),
        'w_br_fnet': nrm((DEPTH, FNET_WIDTH, d), FNET_WIDTH ** -0.5 * DEEPNORM_BETA),
        'w_out': nrm((DEPTH, d, d), d ** -0.5 * DEEPNORM_BETA),
        'ln1_g': 1.0 + nrm((DEPTH, d), 0.01),
        'ln1_b': nrm((DEPTH, d), 0.01),
        'ln2_g': 1.0 + nrm((DEPTH, d), 0.01),
        'ln2_b': nrm((DEPTH, d), 0.01),
        'rg_w': nrm((DEPTH, d, N_GROUPS), d ** -0.5),
        'rg_b': nrm((DEPTH, N_GROUPS), 0.01),
        're_w': nrm((DEPTH, d, N_EXPERTS), d ** -0.5),
        're_b': nrm((DEPTH, N_EXPERTS), 0.01),
        'moe_w1': nrm((DEPTH, N_EXPERTS, d, EXPERT_HIDDEN), d ** -0.5),
        'moe_w3': nrm((DEPTH, N_EXPERTS, d, EXPERT_HIDDEN), d ** -0.5),
        'moe_w2': nrm((DEPTH, N_EXPERTS, EXPERT_HIDDEN, d), EXPERT_HIDDEN ** -0.5 * DEEPNORM_BETA),
    }


def reference(x, c, ctx, c_ctx, w_ada, b_ada, w_in, conv_w, conv_b, hy_w1, hy_b1, hy_w2, hy_b2,
              hy_w3, hy_b3, hy_w4, hy_b4, hy_freq, hy_dbias, attn_sink, w_br_attn, w_br_hyena,
              w_br_fnet, w_out, ln1_g, ln1_b, ln2_g, ln2_b, rg_w, rg_b, re_w, re_b,
              moe_w1, moe_w3, moe_w2):
    b_, s_, d = x.shape
    n_ctx = ctx.shape[1]
    ang_row, ang_col = axial_rope_angles(s_)
    xc = ctx
    for l in range(DEPTH):
        last = l == DEPTH - 1
        mod = (jax.nn.silu(c) @ w_ada[l] + b_ada[l])[:, None, :]
        mod_c = jax.nn.silu(c_ctx) @ w_ada[l] + b_ada[l]
        sh1, sc1, g1, sh2, sc2, g2 = jnp.split(mod, 6, axis=-1)
        csh1, csc1, cg1, csh2, csc2, cg2 = jnp.split(mod_c, 6, axis=-1)
        filt = (hy_w1[l], hy_b1[l], hy_w2[l], hy_b2[l], hy_w3[l], hy_b3[l], hy_w4[l], hy_b4[l], hy_freq[l])

        h = modulate(x, sh1, sc1)
        hc = modulate(xc, csh1, csc1)
        p = h @ w_in[l]
        if last:
            pc_kv = hc @ w_in[l][:, K_OFF:HY_OFF]
        else:
            pc = hc @ w_in[l]
            pc_kv = pc[..., K_OFF:HY_OFF]
        kc = pc_kv[..., :KV_WIDTH].reshape(b_, n_ctx, N_KV_HEADS, HEAD_DIM)
        vc = pc_kv[..., KV_WIDTH:].reshape(b_, n_ctx, N_KV_HEADS, HEAD_DIM)

        q = apply_axial_rope(p[..., Q_OFF:K_OFF].reshape(b_, s_, N_HEADS, HEAD_DIM), ang_row, ang_col)
        k = apply_axial_rope(p[..., K_OFF:V_OFF].reshape(b_, s_, N_KV_HEADS, HEAD_DIM), ang_row, ang_col)
        v = p[..., V_OFF:HY_OFF].reshape(b_, s_, N_KV_HEADS, HEAD_DIM)
        a_lat = latent_window_attention(q, k, v, kc, vc, attn_sink[l])
        y_lat = mixer_output(p, a_lat, conv_w[l], conv_b[l], filt, hy_dbias[l],
                             w_br_attn[l], w_br_hyena[l], w_br_fnet[l], w_out[l])
        x = post_ln(DEEPNORM_ALPHA * x + g1 * y_lat, ln1_g[l], ln1_b[l])
        if not last:
            qc = pc[..., Q_OFF:K_OFF].reshape(b_, n_ctx, N_HEADS, HEAD_DIM)
            a_ctx = context_attention(qc, kc, vc, attn_sink[l])
            y_ctx = mixer_output(pc, a_ctx, conv_w[l], conv_b[l], filt, hy_dbias[l],
                                 w_br_attn[l], w_br_hyena[l], w_br_fnet[l], w_out[l])
            xc = post_ln(DEEPNORM_ALPHA * xc + cg1 * y_ctx, ln1_g[l], ln1_b[l])

        t_lat = modulate(x, sh2, sc2).reshape(b_ * s_, d)
        if last:
            tokens = t_lat
        else:
            tokens = jnp.concatenate([t_lat, modulate(xc, csh2, csc2).reshape(b_ * n_ctx, d)], 0)
        expert_id, weights = hier_route(tokens, rg_w[l], rg_b[l], re_w[l], re_b[l])
        m = moe_forward(tokens, expert_id, weights, moe_w1[l], moe_w3[l], moe_w2[l])
        x = post_ln(DEEPNORM_ALPHA * x + g2 * m[:b_ * s_].reshape(b_, s_, d), ln2_g[l], ln2_b[l])
        if not last:
            xc = post_ln(DEEPNORM_ALPHA * xc + cg2 * m[b_ * s_:].reshape(b_, n_ctx, d), ln2_g[l], ln2_b[l])
    return x
```

```python
import math
from contextlib import ExitStack
import numpy as np
import ml_dtypes
import concourse.bass as bass
import concourse.mybir as mybir
from concourse.bass_utils import run_bass_kernel_spmd

F32 = mybir.dt.float32
BF16 = mybir.dt.bfloat16
I32 = mybir.dt.int32
ALU = mybir.AluOpType
AF = mybir.ActivationFunctionType

NC_ACTIVE = 4
D = 2048
SEQ = 4096
CTX = 256
DEPTH = 4
NTOK = SEQ + CTX
INW = 9728
K_OFF, V_OFF, HY_OFF, FN_OFF, GATE_OFF = 1024, 1280, 1536, 3072, 3584
NEXP = 32
EH = 512
ALPHA = (2 * DEPTH) ** 0.25
LN_EPS = 1e-6
NKC = 4224
TWO_PI = float(2 * np.pi)


class Prog:
    ENG = ('pe', 'act', 'dve', 'pool', 'sp')

    def __init__(self, nc):
        self.nc = nc
        self.q = {e: [] for e in self.ENG}
        self.semh = {}
        self.cnt = {}
        self.waited = {}
        self.res = {}
        self.ninst = 0

    def _sem(self, name):
        if name not in self.semh:
            self.semh[name] = self.nc.alloc_semaphore(name)
            self.cnt[name] = 0
        return self.semh[name]

    def _wait(self, eng, sn, v):
        if sn.startswith('d_'):
            v = max(v, self.cnt[sn])
        if self.waited.get((eng, sn), 0) < v:
            self.waited[(eng, sn)] = v
            sh = self.semh[sn]
            self.q[eng].append(lambda e, sh=sh, v=v: e.wait_ge(sh, v))

    def issue(self, eng, fn, reads=(), writes=(), chan=None):
        deps = {}
        for r in reads:
            st = self.res.get(r)
            if st and st[0]:
                sn, v = st[0]
                deps[sn] = max(deps.get(sn, 0), v)
        for w in writes:
            st = self.res.get(w)
            if st:
                if st[0]:
                    sn, v = st[0]
                    deps[sn] = max(deps.get(sn, 0), v)
                for sn, v in st[1]:
                    deps[sn] = max(deps.get(sn, 0), v)
        attach = None
        need = []
        for sn, v in deps.items():
            if sn == 'p_pe' and eng == 'pe':
                continue
            if sn.startswith('d_'):
                v = max(v, self.cnt[sn])
            if self.waited.get((eng, sn), 0) < v:
                need.append((sn, v))
        if eng == 'pe' and need:
            need.sort(key=lambda t: (t[0].startswith('p_'), t[1]))
            attach = need.pop()
        for sn, v in need:
            self._wait(eng, sn, v)
        if chan is not None:
            sn, inc = 'd_' + chan, 16
        else:
            sn, inc = 'p_' + eng, 1
        sh = self._sem(sn)
        self.cnt[sn] += inc
        tok = (sn, self.cnt[sn])
        if attach is not None:
            asn, av = attach
            self.waited[(eng, asn)] = av
            ash = self.semh[asn]
            self.q[eng].append(lambda e, fn=fn, sh=sh, inc=inc, ash=ash, av=av: fn(e)._wait_ge(ash, av).then_inc(sh, inc))
        else:
            self.q[eng].append(lambda e, fn=fn, sh=sh, inc=inc: fn(e).then_inc(sh, inc))
        for r in reads:
            st = self.res.setdefault(r, [None, []])
            if len(st[1]) > 48:
                mx = {}
                for sn_, v_ in st[1]:
                    mx[sn_] = max(mx.get(sn_, 0), v_)
                st[1] = list(mx.items())
            st[1].append(tok)
        for w in writes:
            self.res[w] = [tok, []]
        self.ninst += 1
        return tok

    def barrier(self):
        for e in self.ENG:
            for sn, c in self.cnt.items():
                if c:
                    self._wait(e, sn, c)
        self.res = {}

    def finish(self):
        self.barrier()
        with self.nc.Block() as block:
            @block.tensor
            def _(e):
                for t in self.q['pe']:
                    t(e)

            @block.scalar
            def _(e):
                for t in self.q['act']:
                    t(e)

            @block.vector
            def _(e):
                for t in self.q['dve']:
                    t(e)

            @block.gpsimd
            def _(e):
                for t in self.q['pool']:
                    t(e)

            @block.sync
            def _(e):
                for t in self.q['sp']:
                    t(e)

    def dma(self, out, in_, reads, writes, chan, eng='sp', **kw):
        return self.issue(eng, lambda e: e.dma_start(out=out, in_=in_, **kw), reads, writes, chan=chan)

    def dmac(self, out, in_, reads, writes, chan, **kw):
        return self.dma(out, in_, reads, writes, chan, eng='pool', **kw)

    def mm(self, out, lhsT, rhs, start, stop, reads, writes):
        return self.issue('pe', lambda e: e.matmul(out, lhsT, rhs, start=start, stop=stop), reads, writes)

    def tr(self, out, in_, ident, reads, writes):
        return self.issue('pe', lambda e: e.transpose(out, in_, ident), reads, writes)

    def act(self, out, in_, func, reads, writes, **kw):
        return self.issue('act', lambda e: e.activation(out, in_, func, **kw), reads, writes)

    def ts(self, out, in0, s1, s2, op0, op1, reads, writes, eng='dve'):
        return self.issue(eng, lambda e: e.tensor_scalar(out, in0, s1, s2, op0, op1), reads, writes)

    def tt(self, out, in0, in1, op, reads, writes, eng='dve'):
        return self.issue(eng, lambda e: e.tensor_tensor(out, in0, in1, op), reads, writes)

    def stt(self, out, in0, scalar, in1, op0, op1, reads, writes, eng='dve'):
        return self.issue(eng, lambda e: e.scalar_tensor_tensor(out, in0, scalar, in1, op0, op1), reads, writes)

    def cp(self, out, in_, reads, writes, eng='dve'):
        if eng == 'act':
            return self.issue('act', lambda e: e.copy(out, in_), reads, writes)
        return self.issue(eng, lambda e: e.tensor_copy(out, in_), reads, writes)

    def memset(self, ap, val, writes, eng='dve'):
        return self.issue(eng, lambda e: e.memset(ap, val), (), writes)

    def recip(self, out, in_, reads, writes):
        return self.issue('dve', lambda e: e.reciprocal(out, in_), reads, writes)


class Scope:
    def __init__(self, nc):
        self.nc = nc
        self.es = ExitStack()

    def __enter__(self):
        self.es.__enter__()
        return self

    def __exit__(self, *a):
        return self.es.__exit__(*a)

    _uid = [0]

    def sb(self, name, shape, dt=F32):
        Scope._uid[0] += 1
        return self.es.enter_context(self.nc.sbuf_tensor("%s_%d" % (name, Scope._uid[0]), list(shape), dt))

    def ps(self, name, shape, dt=F32):
        Scope._uid[0] += 1
        return self.es.enter_context(self.nc.psum_tensor("%s_%d" % (name, Scope._uid[0]), list(shape), dt))


_CONST_CACHE = {}


def _consts():
    if _CONST_CACHE:
        return _CONST_CACHE
    j = np.arange(8192, dtype=np.int64)[:, None]
    k = np.arange(NKC, dtype=np.int64)[None, :]
    ang = (2.0 * np.pi / 8192.0) * ((j * k) % 8192).astype(np.float64)
    cosT = np.cos(ang).astype(ml_dtypes.bfloat16)
    sinT = np.sin(ang).astype(ml_dtypes.bfloat16)
    t = np.arange(SEQ)
    row, col = t // 64, t % 64
    inv = (10000.0 ** (-np.arange(32, dtype=np.float32) / 32)).astype(np.float32)
    ar = row.astype(np.float32)[:, None] * inv[None, :]
    ac = col.astype(np.float32)[:, None] * inv[None, :]
    c128 = np.concatenate([np.cos(ar), np.cos(ar), np.cos(ac), np.cos(ac)], 1)
    s128 = np.concatenate([-np.sin(ar), np.sin(ar), -np.sin(ac), np.sin(ac)], 1)
    ropeC = np.tile(c128, (1, 4)).astype(np.float32)
    ropeS = np.tile(s128, (1, 4)).astype(np.float32)
    def zfeat(n):
        tt = np.linspace(0.0, 1.0, n, dtype=np.float32)[:, None]
        w = (2.0 * math.pi * np.arange(n, dtype=np.float32)[:, None] / n).astype(np.float32)
        bands = np.linspace(1e-4, 15, 16, dtype=np.float32)[None, :]
        z = np.concatenate([tt, np.cos(bands * w), -np.sin(bands * w)], -1).astype(np.float32)
        negt = (-tt[:, 0]).reshape(n // 128, 128).T.copy()
        return np.ascontiguousarray(z.T), np.ascontiguousarray(negt.astype(np.float32))
    zl, ntl = zfeat(SEQ)
    zc, ntc = zfeat(CTX)
    max_decay = math.log(1e-2) / 0.3
    min_decay = math.log(1e-2) / 1.5
    deltas = np.abs(np.linspace(min_decay, max_decay, 512, dtype=np.float32)).astype(np.float32)[None, :]
    def wk(n, nch):
        N = 2 * n
        w = np.zeros(nch * 128, np.float32)
        w[:n + 1] = 2.0 / N
        w[0] = 1.0 / N
        w[n] = 1.0 / N
        return np.ascontiguousarray(w.reshape(nch, 128).T)
    jj = np.arange(128)[:, None]
    qq = np.arange(128)[None, :]
    mprev = np.tile((jj >= qq).astype(np.float32), (1, 4)).astype(ml_dtypes.bfloat16)
    mnext = np.tile((jj <= qq).astype(np.float32), (1, 4)).astype(ml_dtypes.bfloat16)
    _CONST_CACHE.update(dict(
        cosT=cosT, sinT=sinT, ropeC=ropeC, ropeS=ropeS, zl=zl, zc=zc, ntl=ntl, ntc=ntc,
        deltas=deltas, wkl=wk(SEQ, 33), wkc=wk(CTX, 3), mprev=mprev, mnext=mnext,
        ident=np.eye(128, dtype=np.float32)))
    return _CONST_CACHE


CONST_SHAPES = dict(cosT=([8192, NKC], BF16), sinT=([8192, NKC], BF16), ropeC=([SEQ, 512], F32),
                    ropeS=([SEQ, 512], F32), zl=([33, SEQ], F32), zc=([33, CTX], F32),
                    ntl=([128, 32], F32), ntc=([128, 2], F32), deltas=([1, 512], F32),
                    wkl=([128, 33], F32), wkc=([128, 3], F32), mprev=([128, 512], BF16),
                    mnext=([128, 512], BF16), ident=([128, 128], F32))

def weight_shapes(DEPTH):
  return dict(
    w_ada=[DEPTH * D, 6 * D], b_ada=[DEPTH, 6 * D], w_in=[DEPTH * D, INW],
    conv_w=[DEPTH * 3, 1536], conv_b=[DEPTH, 1536], hy_w1=[DEPTH * 33, 64], hy_b1=[DEPTH, 64],
    hy_w2=[DEPTH * 64, 64], hy_b2=[DEPTH, 64], hy_w3=[DEPTH * 64, 64], hy_b3=[DEPTH, 64],
    hy_w4=[DEPTH * 64, 1024], hy_b4=[DEPTH, 1024], hy_freq=[DEPTH * 3, 64], hy_dbias=[DEPTH, 512],
    attn_sink=[DEPTH, 8], w_br_attn=[DEPTH * 1024, D], w_br_hyena=[DEPTH * 512, D],
    w_br_fnet=[DEPTH * 512, D], w_out=[DEPTH * D, D], ln1_g=[DEPTH, D], ln1_b=[DEPTH, D],
    ln2_g=[DEPTH, D], ln2_b=[DEPTH, D], rg_w=[DEPTH * D, 4], rg_b=[DEPTH, 4], re_w=[DEPTH * D, 32],
    re_b=[DEPTH, 32], moe_w1=[DEPTH * NEXP * D, EH], moe_w3=[DEPTH * NEXP * D, EH],
    moe_w2=[DEPTH * NEXP * EH, D])


WEIGHT_SHAPES = weight_shapes(DEPTH)


class _Stop(Exception):
    pass


def build_nc(NB, depth=DEPTH, wdepth=DEPTH, stop=0, tiny=(), substop=0):
    nc = bass.Bass("TRN2", target_bir_lowering=False)
    T = {}
    T['x'] = nc.dram_tensor("x", [NB * SEQ, D], F32, kind="ExternalInput")
    T['ctx'] = nc.dram_tensor("ctx", [NB * CTX, D], F32, kind="ExternalInput")
    T['c'] = nc.dram_tensor("c", [NB * 16, 128], F32, kind="ExternalInput")
    T['c_ctx'] = nc.dram_tensor("c_ctx", [16, 128], F32, kind="ExternalInput")
    for n, s in weight_shapes(wdepth).items():
        T[n] = nc.dram_tensor(n, [128, s[1]] if n in tiny else s, F32, kind="ExternalInput")
    for n, (s, dt) in CONST_SHAPES.items():
        T[n] = nc.dram_tensor(n, s, dt, kind="ExternalInput")
    OUT = nc.dram_tensor("out", [NB * SEQ, D], F32, kind="ExternalOutput")
    X = nc.dram_tensor("Xs", [NTOK, D], F32)
    P = nc.dram_tensor("Ps", [NTOK, INW], F32)
    YB = nc.dram_tensor("YBs", [NTOK, D], F32)
    MOD = nc.dram_tensor("MODs", [2, 6 * D], F32)
    HZ = nc.dram_tensor("HZs", [NTOK, 512], F32)
    HX0 = nc.dram_tensor("HX0s", [NTOK, 512], F32)
    HFR = nc.dram_tensor("HFRs", [NKC, 512], F32)
    HFI = nc.dram_tensor("HFIs", [NKC, 512], F32)
    RN = nc.dram_tensor("RNs", [1, 512], F32)
    TT = nc.dram_tensor("TTs", [9, 128, 16 * 512], BF16)
    GD = nc.dram_tensor("GDs", [NTOK, NEXP], F32)
    MD = nc.dram_tensor("MDs", [NTOK, D], BF16)
    p = Prog(nc)
    _ph = [0]
    _orig_barrier = p.barrier

    def _barrier():
        _orig_barrier()
        _ph[0] += 1
        if stop and _ph[0] >= stop:
            raise _Stop()
    p.barrier = _barrier

    def sub(k):
        if substop == k:
            _orig_barrier()
            raise _Stop()

    def groups(with_ctx=True):
        g = [(i * 512, 4, False) for i in range(8)]
        if with_ctx:
            g.append((SEQ, 2, True))
        return g

    def bc_load(sc, name, src_row_ap, width=D):
        t = sc.sb(name, [128, width])
        p.dma(t[:, :], src_row_ap.partition_broadcast(128), [], [name], 'c')
        return t

    def layer_norm_tile(sc, xt, key, st, mv, rstd):
        for c in range(4):
            p.issue('dve', lambda e, c=c: e.bn_stats(st[:, c, :], xt[:, c * 512:(c + 1) * 512]), [key], ['st%d' % c])
        p.issue('dve', lambda e: e.bn_aggr(mv[:, :], st[:, :, :].rearrange("p a b -> p (a b)")),
                ['st%d' % c for c in range(4)], ['mv'])
        p.act(rstd[:, :], mv[:, 1:2], AF.Sqrt, ['mv'], ['rstd'], bias=LN_EPS, scale=1.0)
        p.recip(rstd[:, :], rstd[:, :], ['rstd'], ['rstd'])
        p.ts(xt[:, :], xt[:, :], mv[:, 0:1], rstd[:, 0:1], ALU.subtract, ALU.mult, [key, 'mv', 'rstd'], [key])

    def transpose_tile(src_bf, src_key, dstT, dst_key, col0, nchunks, ps_t, ident, eng_alt=0):
        for g in range(0, nchunks, 4):
            n = min(4, nchunks - g)
            for j in range(n):
                p.tr(ps_t[:, j, :], src_bf[:, (g + j) * 128:(g + j + 1) * 128], ident[:, :], [src_key, 'ident'], ['ps_t'])
            e = 'act' if (g // 4 + eng_alt) % 2 == 0 else 'dve'
            p.cp(dstT[:, g:g + n, col0:col0 + 128], ps_t[:, 0:n, :], ['ps_t'], [dst_key], eng=e)

    try:
      for b in range(NB):
          for i8 in range(8):
              p.dma(X[i8 * 512:(i8 + 1) * 512, :], T['x'][b * SEQ + i8 * 512:b * SEQ + (i8 + 1) * 512, :], [], ['X'], 'c')
          p.dma(X[SEQ:NTOK, :], T['ctx'][b * CTX:(b + 1) * CTX, :], [], ['X'], 'c')
          p.barrier()
          for l in range(depth):
              last = (l == DEPTH - 1)
              with Scope(nc) as sc:
                  c2 = sc.sb("c2", [32, 128]); cT = sc.sb("cT", [128, 32]); cT2 = sc.sb("cT2", [128, 16, 2])
                  identf = sc.sb("identf", [128, 128])
                  wa = [sc.sb("wa%d" % i, [128, 16, 512]) for i in range(2)]
                  modrow = sc.sb("modrow", [2, 6 * D]); bada = sc.sb("bada", [2, 6 * D])
                  ps_c = sc.ps("ps_c", [128, 32]); ps_m = [sc.ps("ps_m%d" % i, [2, 512]) for i in range(2)]
                  p.dma(identf[:, :], T['ident'][:, :], [], ['identf'], 'c')
                  p.dma(c2[0:16, :], T['c'][b * 16:(b + 1) * 16, :], [], ['c2'], 'c')
                  p.dma(c2[16:32, :], T['c_ctx'][:, :], [], ['c2'], 'c')
                  p.dma(bada[:, :], T['b_ada'][l:l + 1, :].partition_broadcast(2), [], ['bada'], 'c')
                  p.act(c2[:, :], c2[:, :], AF.Silu, ['c2'], ['c2'])
                  p.tr(ps_c[:, :], c2[:, :], identf[0:32, 0:32], ['c2', 'identf'], ['ps_c'])
                  p.cp(cT[:, :], ps_c[:, :], ['ps_c'], ['cT'])
                  p.cp(cT2[:, :, :], cT[:, :].rearrange("p (r k) -> p k r", r=2), ['cT'], ['cT2'])
                  for nch in range(24):
                      wt = wa[nch % 2]; wk_ = 'wa%d' % (nch % 2)
                      p.dma(wt[:, :, :], T['w_ada'][l * D:(l + 1) * D, nch * 512:(nch + 1) * 512]
                            .rearrange("(kc p) n -> p kc n", p=128), [], [wk_], wk_)
                      pm = ps_m[nch % 2]; pk = 'ps_m%d' % (nch % 2)
                      for kc in range(16):
                          p.mm(pm[:, :], cT2[:, kc, :], wt[:, kc, :], kc == 0, kc == 15, ['cT2', wk_], [pk])
                      p.tt(modrow[:, nch * 512:(nch + 1) * 512], pm[:, :], bada[:, nch * 512:(nch + 1) * 512],
                           ALU.add, [pk, 'bada'], ['modrow'])
                  for off in (D, 4 * D):
                      p.ts(modrow[:, off:off + D], modrow[:, off:off + D], 1.0, None, ALU.add, ALU.bypass, ['modrow'], ['modrow'])
                  p.dma(MOD[:, :], modrow[:, :], ['modrow'], ['MOD'], 'o')
                  p.barrier()

              with Scope(nc) as sc:
                  ident = sc.sb("ident", [128, 128], BF16); identf = sc.sb("identf", [128, 128])
                  xt = [sc.sb("xt%d" % i, [128, D]) for i in range(2)]
                  hb = sc.sb("hb", [128, D], BF16)
                  st = sc.sb("st", [128, 4, 6]); mv = sc.sb("mv", [128, 2]); rstd = sc.sb("rstd", [128, 1])
                  hT = sc.sb("hT", [128, 16, 512], BF16)
                  wb = [sc.sb("wb%d" % i, [128, 16, 512], BF16) for i in range(2)]
                  ot = [sc.sb("ot%d" % i, [128, 512]) for i in range(4)]
                  t2 = sc.sb("t2", [128, 512])
                  rc = sc.sb("rc", [128, 4, 512]); rs = sc.sb("rs", [128, 4, 512])
                  ps_t = sc.ps("ps_t", [128, 4, 128], BF16)
                  ps_o = [sc.ps("ps_o%d" % i, [128, 512]) for i in range(4)]
                  p.dma(identf[:, :], T['ident'][:, :], [], ['identf'], 'c')
                  p.cp(ident[:, :], identf[:, :], ['identf'], ['ident'])
                  scb = {}; shb = {}
                  for r, nm in ((0, 'l'), (1, 'c')):
                      scb[r] = bc_load(sc, "scb" + nm, MOD[r:r + 1, D:2 * D])
                      shb[r] = bc_load(sc, "shb" + nm, MOD[r:r + 1, 0:D])
                  cnt = 0
                  for (r0, nt, isc) in groups(True):
                      r = 1 if isc else 0
                      for j in range(nt):
                          x_ = xt[j % 2]; xk = 'xt%d' % (j % 2)
                          p.dma(x_[:, :], X[r0 + j * 128:r0 + (j + 1) * 128, :], ['X'], [xk], xk)
                          layer_norm_tile(sc, x_, xk, st, mv, rstd)
                          p.tt(x_[:, :], x_[:, :], scb[r][:, :], ALU.mult, [xk, 'scb' + ('c' if isc else 'l')], [xk], eng='pool')
                          p.tt(hb[:, :], x_[:, :], shb[r][:, :], ALU.add, [xk, 'shb' + ('c' if isc else 'l')], ['hb'], eng='pool')
                          transpose_tile(hb, 'hb', hT, 'hT', j * 128, 16, ps_t, ident)
                          if not isc:
                              p.dma(rc[:, j, :], T['ropeC'][r0 + j * 128:r0 + (j + 1) * 128, :], [], ['rc'], 'rc')
                              p.dma(rs[:, j, :], T['ropeS'][r0 + j * 128:r0 + (j + 1) * 128, :], [], ['rs'], 'rc')
                      for nch in range(19):
                          wt = wb[nch % 2]; wk_ = 'wb%d' % (nch % 2)
                          p.dmac(wt[:, :, :], T['w_in'][l * D:(l + 1) * D, nch * 512:(nch + 1) * 512]
                                 .rearrange("(kc p) n -> p kc n", p=128), [], [wk_], wk_)
                          for j in range(nt):
                              ps = ps_o[cnt % 4]; pk = 'ps_o%d' % (cnt % 4)
                              o_ = ot[cnt % 4]; ok = 'ot%d' % (cnt % 4)
                              cnt += 1
                              for kc in range(16):
                                  p.mm(ps[:, :], hT[:, kc, j * 128:(j + 1) * 128], wt[:, kc, :], kc == 0, kc == 15, ['hT', wk_], [pk])
                              rope_cols = 0
                              if not isc:
                                  rope_cols = 512 if nch < 2 else (256 if nch == 2 else 0)
                              if nch >= 7:
                                  p.act(o_[:, :], ps[:, :], AF.Sigmoid, [pk], [ok])
                              elif rope_cols:
                                  w_ = rope_cols
                                  p.tt(o_[:, 0:w_], ps[:, 0:w_], rc[:, j, 0:w_], ALU.mult, [pk, 'rc'], [ok])
                                  pv = ps[:, 0:w_].rearrange("p (b h f) -> p b h f", h=2, f=32)
                                  sv = rs[:, j, 0:w_].rearrange("p (b h f) -> p b h f", h=2, f=32)
                                  tv = t2[:, 0:w_].rearrange("p (b h f) -> p b h f", h=2, f=32)
                                  p.tt(tv[:, :, 0, :], pv[:, :, 1, :], sv[:, :, 0, :], ALU.mult, [pk, 'rs'], ['t2'])
                                  p.tt(tv[:, :, 1, :], pv[:, :, 0, :], sv[:, :, 1, :], ALU.mult, [pk, 'rs'], ['t2'])
                                  p.tt(o_[:, 0:w_], o_[:, 0:w_], t2[:, 0:w_], ALU.add, [ok, 't2'], [ok])
                                  if w_ < 512:
                                      p.cp(o_[:, w_:512], ps[:, w_:512], [pk], [ok], eng='act')
                              else:
                                  p.cp(o_[:, :], ps[:, :], [pk], [ok], eng='act')
                              p.dma(P[r0 + j * 128:r0 + (j + 1) * 128, nch * 512:(nch + 1) * 512], o_[:, :], [ok], ['P'], ok)
                  p.barrier()

              with Scope(nc) as sc:
                  ident = sc.sb("ident", [128, 128], BF16); identf = sc.sb("identf", [128, 128])
                  KT = sc.sb("KT", [128, 2, 34, 128], BF16)
                  VE = sc.sb("VE", [128, 34, 2, 130], BF16)
                  kv = [sc.sb("kv%d" % i, [128, 512], BF16) for i in range(2)]
                  qb = [sc.sb("qb%d" % i, [128, 1024], BF16) for i in range(2)]
                  qT = sc.sb("qT", [128, 8, 128], BF16)
                  ET = sc.sb("ET", [128, 5, 512], BF16)
                  mk = {'p': sc.sb("mprev", [128, 512], BF16), 'n': sc.sb("mnext", [128, 512], BF16)}
                  sk = sc.sb("sk", [128, 8]); rden = sc.sb("rden", [128, 1]); osb = sc.sb("osb", [128, 130])
                  yt = [sc.sb("yt%d" % i, [128, 1024]) for i in range(2)]
                  ps_t = sc.ps("ps_t", [128, 4, 128], BF16)
                  ps_s = [sc.ps("ps_s%d" % i, [128, 512]) for i in range(2)]
                  ps_a = [sc.ps("ps_a%d" % i, [128, 512]) for i in range(4)]
                  p.dma(identf[:, :], T['ident'][:, :], [], ['identf'], 'c')
                  p.cp(ident[:, :], identf[:, :], ['identf'], ['ident'])
                  p.dma(mk['p'][:, :], T['mprev'][:, :], [], ['mk'], 'c')
                  p.dma(mk['n'][:, :], T['mnext'][:, :], [], ['mk'], 'c')
                  p.dma(sk[:, :], T['attn_sink'][l:l + 1, :].partition_broadcast(128), [], ['sk'], 'c')
                  p.act(sk[:, :], sk[:, :], AF.Exp, ['sk'], ['sk'])
                  p.memset(VE[:, :, :, :], 1.0, ['VE'])
                  sub(1)
                  for c in range(34):
                      k_ = kv[c % 2]; kk = 'kv%d' % (c % 2)
                      p.dmac(k_[:, :], P[c * 128:(c + 1) * 128, K_OFF:K_OFF + 512], ['P'], [kk], kk)
                      for g in range(2):
                          p.tr(ps_t[:, g, :], k_[:, g * 128:(g + 1) * 128], ident[:, :], [kk, 'ident'], ['ps_t'])
                      p.cp(KT[:, :, c, :], ps_t[:, 0:2, :], ['ps_t'], ['KT'], eng='act')
                      p.cp(VE[:, c, :, 0:128], k_[:, 256:512].rearrange("p (g d) -> p g d", g=2), [kk], ['VE'])
                  sub(2)
                  nqb = 32 if last else 34
                  for n in range(nqb):
                      q_ = qb[n % 2]; qk = 'qb%d' % (n % 2)
                      y_ = yt[n % 2]; yk = 'yt%d' % (n % 2)
                      p.dmac(q_[:, :], P[n * 128:(n + 1) * 128, 0:1024], ['P'], [qk], qk)
                      transpose_tile(q_, qk, qT, 'qT', 0, 8, ps_t, ident)
                      sub(100)
                      if n < 32:
                          chunks = [(c, m) for (c, m) in ((n - 1, 'p'), (n, None), (n + 1, 'n')) if 0 <= c < 32]
                          chunks += [(32, None), (33, None)]
                      else:
                          chunks = [(32, None), (33, None)]
                      for g in range(2):
                          for ci, (c, m) in enumerate(chunks):
                              p.mm(ps_s[ci % 2][:, :], KT[:, g, c, :], qT[:, 4 * g:4 * g + 4, :].rearrange("p h q -> p (h q)"),
                                   True, True, ['KT', 'qT'], ['ps_s%d' % (ci % 2)])
                              p.act(ET[:, ci, :], ps_s[ci % 2][:, :], AF.Exp, ['ps_s%d' % (ci % 2)], ['ET%d' % ci], scale=128 ** -0.5)
                              if m:
                                  p.tt(ET[:, ci, :], ET[:, ci, :], mk[m][:, :], ALU.mult, ['ET%d' % ci, 'mk'], ['ET%d' % ci])
                          sub(101)
                          for h in range(4):
                              pa = ps_a[h]; pak = 'ps_a%d' % h
                              for ci, (c, m) in enumerate(chunks):
                                  p.mm(pa[:, 0:130], ET[:, ci, h * 128:(h + 1) * 128], VE[:, c, g, 0:130],
                                       ci == 0, ci == len(chunks) - 1, ['ET%d' % ci, 'VE'], [pak])
                              sub(102)
                              hh = 4 * g + h
                              p.cp(osb[:, :], pa[:, 0:130], [pak], ['osb'], eng='act')
                              p.tt(rden[:, :], osb[:, 128:129], sk[:, hh:hh + 1], ALU.add, ['osb', 'sk'], ['rden'])
                              p.recip(rden[:, :], rden[:, :], ['rden'], ['rden'])
                              p.ts(y_[:, hh * 128:(hh + 1) * 128], osb[:, 0:128], rden[:, 0:1], None, ALU.mult, ALU.bypass,
                                   ['osb', 'rden'], [yk])
                              sub(103 + h + 10 * g)
                      p.dma(YB[n * 128:(n + 1) * 128, 0:1024], y_[:, :], [yk], ['YB'], 'o')
                      sub(3 + n)
                  p.barrier()

              seqs = [(0, SEQ, 1, 33, 'zl', 'ntl', 'wkl')]
              if not last:
                  seqs.append((SEQ, CTX, 16, 3, 'zc', 'ntc', 'wkc'))

              with Scope(nc) as sc:
                  cw = [bc_load(sc, "cw%d" % jx, T['conv_w'][l * 3 + jx:l * 3 + jx + 1, :], 1536) for jx in range(3)]
                  cbb = bc_load(sc, "cbb", T['conv_b'][l:l + 1, :], 1536)
                  um = [sc.sb("um%d" % i, [128, 1536]) for i in range(2)]
                  u0 = [sc.sb("u0%d" % i, [128, 1536]) for i in range(2)]
                  up = [sc.sb("up%d" % i, [128, 1536]) for i in range(2)]
                  zt = [sc.sb("zt%d" % i, [128, 512]) for i in range(2)]
                  it = 0
                  for (r0, n, _, _, _, _, _) in seqs:
                      for ti in range(n // 128):
                          a = it % 2; it += 1
                          rr = r0 + ti * 128
                          ks = ['um%d' % a, 'u0%d' % a, 'up%d' % a]
                          p.dma(u0[a][:, :], P[rr:rr + 128, HY_OFF:HY_OFF + 1536], ['P'], [ks[1]], 'u' + ks[1])
                          if ti == 0:
                              p.memset(um[a][:, :], 0.0, [ks[0]])
                              p.dma(um[a][1:128, :], P[rr:rr + 127, HY_OFF:HY_OFF + 1536], ['P'], [ks[0]], 'u' + ks[0])
                          else:
                              p.dma(um[a][:, :], P[rr - 1:rr + 127, HY_OFF:HY_OFF + 1536], ['P'], [ks[0]], 'u' + ks[0])
                          if ti == n // 128 - 1:
                              p.memset(up[a][:, :], 0.0, [ks[2]])
                              p.dma(up[a][0:127, :], P[rr + 1:rr + 128, HY_OFF:HY_OFF + 1536], ['P'], [ks[2]], 'u' + ks[2])
                          else:
                              p.dma(up[a][:, :], P[rr + 1:rr + 129, HY_OFF:HY_OFF + 1536], ['P'], [ks[2]], 'u' + ks[2])
                          p.tt(um[a][:, :], um[a][:, :], cw[0][:, :], ALU.mult, [ks[0], 'cw0'], [ks[0]])
                          p.tt(u0[a][:, :], u0[a][:, :], cw[1][:, :], ALU.mult, [ks[1], 'cw1'], [ks[1]], eng='pool')
                          p.tt(up[a][:, :], up[a][:, :], cw[2][:, :], ALU.mult, [ks[2], 'cw2'], [ks[2]])
                          p.tt(um[a][:, :], um[a][:, :], cbb[:, :], ALU.add, [ks[0], 'cbb'], [ks[0]], eng='pool')
                          p.tt(u0[a][:, :], u0[a][:, :], up[a][:, :], ALU.add, [ks[1], ks[2]], [ks[1]])
                          p.tt(u0[a][:, :], u0[a][:, :], um[a][:, :], ALU.add, [ks[1], ks[0]], [ks[1]])
                          p.tt(zt[a][:, :], u0[a][:, 512:1024], u0[a][:, 1024:1536], ALU.mult, [ks[1]], ['zt%d' % a])
                          p.dma(HZ[rr:rr + 128, :], zt[a][:, :], ['zt%d' % a], ['HZ'], 'o')
                          p.dma(HX0[rr:rr + 128, :], u0[a][:, 0:512], [ks[1]], ['HX0'], 'o')
                  p.barrier()

              for (r0, n, rstride, nkc, zname, ntname, wkname) in seqs:
                  nch_s = n // 128
                  cosv = T['cosT'].ap().rearrange("(r s) k -> r s k", s=rstride)[:, 0, :]
                  sinv = T['sinT'].ap().rearrange("(r s) k -> r s k", s=rstride)[:, 0, :]
                  with Scope(nc) as sc:
                      zT = sc.sb("zT", [33, n]); h1 = sc.sb("h1", [64, n]); h2 = sc.sb("h2", [64, n])
                      w1 = sc.sb("w1", [33, 64]); w2 = sc.sb("w2", [64, 64]); w3 = sc.sb("w3", [64, 64])
                      w4 = sc.sb("w4", [64, 1024])
                      fr = sc.sb("fr", [64, 3]); bb = sc.sb("bb", [64, 3]); frb = sc.sb("frb", [64, 3]); fr2 = sc.sb("fr2", [64, 3])
                      pre = sc.sb("pre", [64, 512]); ki = sc.sb("ki", [64, 512], I32); kf = sc.sb("kf", [64, 512])
                      b4 = bc_load(sc, "b4", T['hy_b4'][l:l + 1, :], 1024)
                      dl = bc_load(sc, "dl", T['deltas'][0:1, :], 512)
                      negt = sc.sb("negt", [128, nch_s]); dec = sc.sb("dec", [128, 512])
                      ff = sc.sb("ff", [128, 512]); fb_ = sc.sb("fb_", [128, 512]); ab = sc.sb("ab", [128, 512]); ab2 = sc.sb("ab2", [128, 512])
                      FS = sc.sb("FS", [128, nch_s, 512], BF16); FD = sc.sb("FD", [128, nch_s, 512], BF16)
                      onec = sc.sb("onec", [128, 1]); rn = sc.sb("rn", [1, 512])
                      tc_ = [sc.sb("tc%d" % i, [128, nch_s, 128], BF16) for i in range(2)]
                      tsn = [sc.sb("tsn%d" % i, [128, nch_s, 128], BF16) for i in range(2)]
                      hre = sc.sb("hre", [128, 512]); him = sc.sb("him", [128, 512])
                      ps_h = sc.ps("ps_h", [64, 512]); ps_f = [sc.ps("ps_f%d" % i, [128, 512]) for i in range(2)]
                      ps_n = sc.ps("ps_n", [1, 512]); ps_r = sc.ps("ps_r", [128, 512]); ps_i = sc.ps("ps_i", [128, 512])
                      p.dma(zT[:, :], T[zname][:, :], [], ['zT'], 'c')
                      p.dma(w1[:, :], T['hy_w1'][l * 33:(l + 1) * 33, :], [], ['w1'], 'c')
                      p.dma(w2[:, :], T['hy_w2'][l * 64:(l + 1) * 64, :], [], ['w2'], 'c')
                      p.dma(w3[:, :], T['hy_w3'][l * 64:(l + 1) * 64, :], [], ['w3'], 'c')
                      p.dma(w4[:, :], T['hy_w4'][l * 64:(l + 1) * 64, :], [], ['w4'], 'c')
                      p.dma(fr[:, :], T['hy_freq'][l * 3:(l + 1) * 3, :].rearrange("a f -> f a"), [], ['fr'], 'c',
                            allow_slow_non_contiguous=True)
                      for i, nm in enumerate(('hy_b1', 'hy_b2', 'hy_b3')):
                          p.dma(bb[:, i:i + 1], T[nm][l:l + 1, :].rearrange("a f -> f a"), [], ['bb'], 'c',
                                allow_slow_non_contiguous=True)
                      p.dma(negt[:, :], T[ntname][:, :], [], ['negt'], 'c')
                      p.memset(onec[:, :], 1.0, ['onec'])
                      p.tt(frb[:, :], fr[:, :], bb[:, :], ALU.mult, ['fr', 'bb'], ['frb'])
                      p.ts(fr2[:, :], fr[:, :], 1.0 / TWO_PI, None, ALU.mult, ALU.bypass, ['fr'], ['fr2'])
                      p.ts(frb[:, :], frb[:, :], 1.0 / TWO_PI, None, ALU.mult, ALU.bypass, ['frb'], ['frb'])
                      for li, (wt, src, dst, kdim, sk_, dk_) in enumerate(((w1, zT, h1, 33, 'zT', 'h1b'), (w2, h1, h2, 64, 'h1b', 'h2b'),
                                                                           (w3, h2, h1, 64, 'h2b', 'h1b'))):
                          for cc in range(0, n, 512):
                              wd = min(512, n - cc)
                              p.mm(ps_h[:, 0:wd], wt[0:kdim, :], src[0:kdim, cc:cc + wd], True, True,
                                   ['w%d' % (li + 1), sk_], ['ps_h'])
                              p.ts(pre[:, 0:wd], ps_h[:, 0:wd], fr2[:, li:li + 1], frb[:, li:li + 1], ALU.mult, ALU.add,
                                   ['ps_h', 'fr2', 'frb'], ['pre'])
                              p.cp(ki[:, 0:wd], pre[:, 0:wd], ['pre'], ['ki'])
                              p.cp(kf[:, 0:wd], ki[:, 0:wd], ['ki'], ['kf'])
                              p.tt(pre[:, 0:wd], pre[:, 0:wd], kf[:, 0:wd], ALU.subtract, ['pre', 'kf'], ['pre'])
                              p.act(dst[:, cc:cc + wd], pre[:, 0:wd], AF.Sin, ['pre'], [dk_], scale=TWO_PI)
                      hl = h1
                      for jc in range(nch_s):
                          p.act(dec[:, :], dl[:, :], AF.Exp, ['dl', 'negt'], ['dec'], scale=negt[:, jc:jc + 1])
                          for fbi, dst in ((0, ff), (1, fb_)):
                              pf = ps_f[fbi]; pk = 'ps_f%d' % fbi
                              p.mm(pf[:, :], hl[:, jc * 128:(jc + 1) * 128], w4[:, fbi * 512:(fbi + 1) * 512], True, True, ['h1b', 'w4'], [pk])
                              dk = 'ff' if fbi == 0 else 'fb_'
                              p.tt(dst[:, :], pf[:, :], b4[:, fbi * 512:(fbi + 1) * 512], ALU.add, [pk, 'b4'], [dk])
                              p.tt(dst[:, :], dst[:, :], dec[:, :], ALU.mult, [dk, 'dec'], [dk])
                          if jc == 0:
                              p.memset(fb_[0:1, :], 0.0, ['fb_'])
                          p.tt(FS[:, jc, :], ff[:, :], fb_[:, :], ALU.add, ['ff', 'fb_'], ['FS'], eng='pool')
                          p.tt(FD[:, jc, :], fb_[:, :], ff[:, :], ALU.subtract, ['ff', 'fb_'], ['FD'], eng='pool')
                          p.act(ab[:, :], ff[:, :], AF.Abs, ['ff'], ['ab'])
                          p.act(ab2[:, :], fb_[:, :], AF.Abs, ['fb_'], ['ab2'])
                          p.tt(ab[:, :], ab[:, :], ab2[:, :], ALU.add, ['ab', 'ab2'], ['ab'])
                          p.mm(ps_n[:, :], onec[:, :], ab[:, :], jc == 0, jc == nch_s - 1, ['onec', 'ab'], ['ps_n'])
                      p.ts(rn[:, :], ps_n[:, :], 1e-6, None, ALU.add, ALU.bypass, ['ps_n'], ['rn'])
                      p.recip(rn[:, :], rn[:, :], ['rn'], ['rn'])
                      p.dma(RN[:, :], rn[:, :], ['rn'], ['RN'], 'o')
                      for kc in range(nkc):
                          a = kc % 2
                          p.dma(tc_[a][:, :, :], cosv[0:n, kc * 128:(kc + 1) * 128].rearrange("(jc p) k -> p jc k", p=128),
                                [], ['tc%d' % a], 'tc%d' % a)
                          p.dma(tsn[a][:, :, :], sinv[0:n, kc * 128:(kc + 1) * 128].rearrange("(jc p) k -> p jc k", p=128),
                                [], ['tsn%d' % a], 'tsn%d' % a)
                          for jc in range(nch_s):
                              p.mm(ps_r[:, :], tc_[a][:, jc, :], FS[:, jc, :], jc == 0, jc == nch_s - 1, ['tc%d' % a, 'FS'], ['ps_r'])
                          for jc in range(nch_s):
                              p.mm(ps_i[:, :], tsn[a][:, jc, :], FD[:, jc, :], jc == 0, jc == nch_s - 1, ['tsn%d' % a, 'FD'], ['ps_i'])
                          p.cp(hre[:, :], ps_r[:, :], ['ps_r'], ['hre'], eng='act')
                          p.cp(him[:, :], ps_i[:, :], ['ps_i'], ['him'])
                          p.dma(HFR[kc * 128:(kc + 1) * 128, :], hre[:, :], ['hre'], ['HFR'], 'o')
                          p.dma(HFI[kc * 128:(kc + 1) * 128, :], him[:, :], ['him'], ['HFI'], 'o')
                      p.barrier()

                  with Scope(nc) as sc:
                      ZS = sc.sb("ZS", [128, nch_s, 512], BF16)
                      YR = sc.sb("YR", [128, nkc, 512], BF16); YI = sc.sb("YI", [128, nkc, 512], BF16)
                      tc_ = [sc.sb("tc%d" % i, [128, 33, 128], BF16) for i in range(2)]
                      tsn = [sc.sb("tsn%d" % i, [128, 33, 128], BF16) for i in range(2)]
                      hre = sc.sb("hre", [128, 512]); him = sc.sb("him", [128, 512])
                      zc_ = sc.sb("zc_", [128, 512]); zs_ = sc.sb("zs_", [128, 512]); t1 = sc.sb("t1", [128, 512]); t2 = sc.sb("t2", [128, 512])
                      wk = sc.sb("wk", [128, nkc])
                      rnb = bc_load(sc, "rnb", RN[0:1, :], 512)
                      dbb = bc_load(sc, "dbb", T['hy_dbias'][l:l + 1, :], 512)
                      zf = [sc.sb("zf%d" % i, [128, 512]) for i in range(2)]
                      x0 = [sc.sb("x0%d" % i, [128, 512]) for i in range(2)]
                      yo = [sc.sb("yo%d" % i, [128, 512]) for i in range(2)]
                      ps_r = sc.ps("ps_r", [128, 512]); ps_i = sc.ps("ps_i", [128, 512])
                      ps_y = [sc.ps("ps_y%d" % i, [128, 512]) for i in range(2)]
                      p.dma(wk[:, :], T[wkname][:, :], [], ['wk'], 'c')
                      p.dmac(ZS[:, :, :], HZ[r0:r0 + n, :].rearrange("(jc p) c -> p jc c", p=128), ['HZ'], ['ZS'], 'zs')
                      for kc in range(nkc):
                          a = kc % 2
                          p.dma(tc_[a][:, 0:nch_s, :], cosv[0:n, kc * 128:(kc + 1) * 128].rearrange("(jc p) k -> p jc k", p=128),
                                [], ['tc%d' % a], 'tc%d' % a)
                          p.dma(tsn[a][:, 0:nch_s, :], sinv[0:n, kc * 128:(kc + 1) * 128].rearrange("(jc p) k -> p jc k", p=128),
                                [], ['tsn%d' % a], 'tsn%d' % a)
                          p.dma(hre[:, :], HFR[kc * 128:(kc + 1) * 128, :], ['HFR'], ['hre'], 'hf')
                          p.dma(him[:, :], HFI[kc * 128:(kc + 1) * 128, :], ['HFI'], ['him'], 'hf')
                          for jc in range(nch_s):
                              p.mm(ps_r[:, :], tc_[a][:, jc, :], ZS[:, jc, :], jc == 0, jc == nch_s - 1, ['tc%d' % a, 'ZS'], ['ps_r'])
                          for jc in range(nch_s):
                              p.mm(ps_i[:, :], tsn[a][:, jc, :], ZS[:, jc, :], jc == 0, jc == nch_s - 1, ['tsn%d' % a, 'ZS'], ['ps_i'])
                          p.cp(zc_[:, :], ps_r[:, :], ['ps_r'], ['zc_'], eng='act')
                          p.cp(zs_[:, :], ps_i[:, :], ['ps_i'], ['zs_'], eng='act')
                          p.tt(t1[:, :], hre[:, :], zc_[:, :], ALU.mult, ['hre', 'zc_'], ['t1'])
                          p.tt(t2[:, :], him[:, :], zs_[:, :], ALU.mult, ['him', 'zs_'], ['t2'], eng='pool')
                          p.tt(t1[:, :], t1[:, :], t2[:, :], ALU.add, ['t1', 't2'], ['t1'])
                          p.ts(YR[:, kc, :], t1[:, :], wk[:, kc:kc + 1], None, ALU.mult, ALU.bypass, ['t1', 'wk'], ['YR'])
                          p.tt(t1[:, :], hre[:, :], zs_[:, :], ALU.mult, ['hre', 'zs_'], ['t1'])
                          p.tt(t2[:, :], him[:, :], zc_[:, :], ALU.mult, ['him', 'zc_'], ['t2'], eng='pool')
                          p.tt(t1[:, :], t1[:, :], t2[:, :], ALU.subtract, ['t1', 't2'], ['t1'])
                          p.ts(YI[:, kc, :], t1[:, :], wk[:, kc:kc + 1], None, ALU.mult, ALU.bypass, ['t1', 'wk'], ['YI'])
                      for tcx in range(nch_s):
                          a = tcx % 2
                          p.dma(tc_[a][:, 0:nkc, :], cosv[0:nkc * 128, tcx * 128:(tcx + 1) * 128].rearrange("(kc p) t -> p kc t", p=128),
                                [], ['tc%d' % a], 'tc%d' % a)
                          p.dma(tsn[a][:, 0:nkc, :], sinv[0:nkc * 128, tcx * 128:(tcx + 1) * 128].rearrange("(kc p) t -> p kc t", p=128),
                                [], ['tsn%d' % a], 'tsn%d' % a)
                          rr = r0 + tcx * 128
                          p.dma(zf[a][:, :], HZ[rr:rr + 128, :], ['HZ'], ['zf%d' % a], 'zf%d' % a)
                          p.dma(x0[a][:, :], HX0[rr:rr + 128, :], ['HX0'], ['x0%d' % a], 'zf%d' % a)
                          py = ps_y[a]; pk = 'ps_y%d' % a
                          for kc in range(nkc):
                              p.mm(py[:, :], tc_[a][:, kc, :], YR[:, kc, :], kc == 0, False, ['tc%d' % a, 'YR'], [pk])
                          for kc in range(nkc):
                              p.mm(py[:, :], tsn[a][:, kc, :], YI[:, kc, :], False, kc == nkc - 1, ['tsn%d' % a, 'YI'], [pk])
                          o_ = yo[a]; ok = 'yo%d' % a
                          p.tt(o_[:, :], py[:, :], rnb[:, :], ALU.mult, [pk, 'rnb'], [ok])
                          p.tt(zf[a][:, :], zf[a][:, :], dbb[:, :], ALU.mult, ['zf%d' % a, 'dbb'], ['zf%d' % a], eng='pool')
                          p.tt(o_[:, :], o_[:, :], zf[a][:, :], ALU.add, [ok, 'zf%d' % a], [ok])
                          p.tt(o_[:, :], o_[:, :], x0[a][:, :], ALU.mult, [ok, 'x0%d' % a], [ok])
                          p.dma(YB[rr:rr + 128, 1024:1536], o_[:, :], [ok], ['YB'], 'o')
                      p.barrier()

              for (r0, n, rstride, nkc, zname, ntname, wkname) in seqs:
                  nch_s = n // 128
                  rs2 = 8192 // n
                  cosn = T['cosT'].ap().rearrange("(r s) k -> r s k", s=rs2)[:, 0, :]
                  sinn = T['sinT'].ap().rearrange("(r s) k -> r s k", s=rs2)[:, 0, :]
                  cosc = T['cosT'].ap().rearrange("(r s) k -> r s k", s=64)[:, 0, :]
                  sinc = T['sinT'].ap().rearrange("(r s) k -> r s k", s=64)[:, 0, :]
                  with Scope(nc) as sc:
                      ident = sc.sb("ident", [128, 128], BF16); identf = sc.sb("identf", [128, 128])
                      Cc = sc.sb("Cc", [128, 128], BF16); Sc = sc.sb("Sc", [128, 128], BF16)
                      ub = [sc.sb("ub%d" % i, [128, 512], BF16) for i in range(2)]
                      UT = sc.sb("UT", [128, 4, 128], BF16)
                      AS = sc.sb("AS", [128, nch_s, 512], BF16); BS = sc.sb("BS", [128, nch_s, 512], BF16)
                      tc_ = [sc.sb("tc%d" % i, [128, nch_s, 128], BF16) for i in range(2)]
                      tsn = [sc.sb("tsn%d" % i, [128, nch_s, 128], BF16) for i in range(2)]
                      yo = [sc.sb("yo%d" % i, [128, 512]) for i in range(2)]
                      ps_t = sc.ps("ps_t", [128, 4, 128], BF16)
                      ps_ab = [sc.ps("ps_ab%d" % i, [128, 512]) for i in range(4)]
                      ps_y = [sc.ps("ps_y%d" % i, [128, 512]) for i in range(2)]
                      p.dma(identf[:, :], T['ident'][:, :], [], ['identf'], 'c')
                      p.cp(ident[:, :], identf[:, :], ['identf'], ['ident'])
                      p.dma(Cc[:, :], cosc[0:128, 0:128], [], ['Cc'], 'c')
                      p.dma(Sc[:, :], sinc[0:128, 0:128], [], ['Sc'], 'c')
                      for ti in range(nch_s):
                          a = ti % 2
                          rr = r0 + ti * 128
                          p.dmac(ub[a][:, :], P[rr:rr + 128, FN_OFF:FN_OFF + 512], ['P'], ['ub%d' % a], 'ub%d' % a)
                          transpose_tile(ub[a], 'ub%d' % a, UT, 'UT', 0, 4, ps_t, ident)
                          for g in range(4):
                              pa_ = ps_ab[(2 * g) % 4]; pb_ = ps_ab[(2 * g + 1) % 4]
                              p.mm(pa_[:, 0:128], UT[:, g, :], Cc[:, :], True, True, ['UT', 'Cc'], ['ps_ab%d' % ((2 * g) % 4)])
                              p.mm(pb_[:, 0:128], UT[:, g, :], Sc[:, :], True, True, ['UT', 'Sc'], ['ps_ab%d' % ((2 * g + 1) % 4)])
                              p.cp(AS[:, ti, g * 128:(g + 1) * 128], pa_[:, 0:128], ['ps_ab%d' % ((2 * g) % 4)], ['AS'], eng='act')
                              p.ts(BS[:, ti, g * 128:(g + 1) * 128], pb_[:, 0:128], -1.0, None, ALU.mult, ALU.bypass,
                                   ['ps_ab%d' % ((2 * g + 1) % 4)], ['BS'])
                      scale = float((n * 128.0) ** -0.5)
                      for tcx in range(nch_s):
                          a = tcx % 2
                          p.dma(tc_[a][:, :, :], cosn[0:n, tcx * 128:(tcx + 1) * 128].rearrange("(jc p) k -> p jc k", p=128),
                                [], ['tc%d' % a], 'tc%d' % a)
                          p.dma(tsn[a][:, :, :], sinn[0:n, tcx * 128:(tcx + 1) * 128].rearrange("(jc p) k -> p jc k", p=128),
                                [], ['tsn%d' % a], 'tsn%d' % a)
                          py = ps_y[a]; pk = 'ps_y%d' % a
                          for jc in range(nch_s):
                              p.mm(py[:, :], tc_[a][:, jc, :], AS[:, jc, :], jc == 0, False, ['tc%d' % a, 'AS'], [pk])
                          for jc in range(nch_s):
                              p.mm(py[:, :], tsn[a][:, jc, :], BS[:, jc, :], False, jc == nch_s - 1, ['tsn%d' % a, 'BS'], [pk])
                          p.act(yo[a][:, :], py[:, :], AF.Copy, [pk], ['yo%d' % a], scale=scale)
                          rr = r0 + tcx * 128
                          p.dma(YB[rr:rr + 128, 1536:2048], yo[a][:, :], ['yo%d' % a], ['YB'], 'o')
                      p.barrier()

              with Scope(nc) as sc:
                  ident = sc.sb("ident", [128, 128], BF16); identf = sc.sb("identf", [128, 128])
                  ybin = [sc.sb("ybin%d" % i, [128, D], BF16) for i in range(2)]
                  yT = sc.sb("yT", [128, 16, 512], BF16)
                  wb = [sc.sb("wb%d" % i, [128, 16, 512], BF16) for i in range(2)]
                  gt = [sc.sb("gt%d" % i, [128, 3, 512]) for i in range(2)]
                  yv = sc.sb("yv", [128, 512]); tv = sc.sb("tv", [128, 512])
                  ybm = [sc.sb("ybm%d" % i, [128, D], BF16) for i in range(4)]
                  rt = [sc.sb("rt%d" % i, [128, D]) for i in range(4)]
                  st = sc.sb("st", [128, 4, 6]); mv = sc.sb("mv", [128, 2]); rstd = sc.sb("rstd", [128, 1])
                  lg = bc_load(sc, "lg", T['ln1_g'][l:l + 1, :]); lb = bc_load(sc, "lb", T['ln1_b'][l:l + 1, :])
                  g1 = {0: bc_load(sc, "g1l", MOD[0:1, 2 * D:3 * D])}
                  if not last:
                      g1[1] = bc_load(sc, "g1c", MOD[1:2, 2 * D:3 * D])
                  ps_t = sc.ps("ps_t", [128, 4, 128], BF16)
                  ps_1 = sc.ps("ps_1", [128, 512]); ps_2 = sc.ps("ps_2", [128, 512]); ps_3 = sc.ps("ps_3", [128, 512])
                  ps_o = [sc.ps("ps_o%d" % i, [128, 512]) for i in range(2)]
                  p.dma(identf[:, :], T['ident'][:, :], [], ['identf'], 'c')
                  p.cp(ident[:, :], identf[:, :], ['identf'], ['ident'])
                  gcnt = 0
                  for (r0, nt, isc) in groups(not last):
                      r = 1 if isc else 0
                      for j in range(nt):
                          a = j % 2
                          p.dmac(ybin[a][:, :], YB[r0 + j * 128:r0 + (j + 1) * 128, :], ['YB'], ['ybin%d' % a], 'ybin%d' % a)
                          transpose_tile(ybin[a], 'ybin%d' % a, yT, 'yT', j * 128, 16, ps_t, ident)
                          p.dma(rt[j][:, :], X[r0 + j * 128:r0 + (j + 1) * 128, :], ['X'], ['rt%d' % j], 'rt%d' % j)
                      for nch in range(4):
                          wt = wb[nch % 2]; wk_ = 'wb%d' % (nch % 2)
                          cs = slice(nch * 512, (nch + 1) * 512)
                          p.dmac(wt[:, 0:8, :], T['w_br_attn'][l * 1024:(l + 1) * 1024, cs].rearrange("(kc p) n -> p kc n", p=128), [], [wk_], wk_)
                          p.dmac(wt[:, 8:12, :], T['w_br_hyena'][l * 512:(l + 1) * 512, cs].rearrange("(kc p) n -> p kc n", p=128), [], [wk_], wk_)
                          p.dmac(wt[:, 12:16, :], T['w_br_fnet'][l * 512:(l + 1) * 512, cs].rearrange("(kc p) n -> p kc n", p=128), [], [wk_], wk_)
                          for j in range(nt):
                              a = gcnt % 2; gcnt += 1
                              rows = slice(r0 + j * 128, r0 + (j + 1) * 128)
                              p.dma(gt[a][:, :, :], P[rows, GATE_OFF:GATE_OFF + 3 * D].rearrange("p (g d) -> p g d", g=3)[:, :, cs],
                                    ['P'], ['gt%d' % a], 'gt%d' % a)
                              tk = slice(j * 128, (j + 1) * 128)
                              for kc in range(8):
                                  p.mm(ps_1[:, :], yT[:, kc, tk], wt[:, kc, :], kc == 0, kc == 7, ['yT', wk_], ['ps_1'])
                              for kc in range(8, 12):
                                  p.mm(ps_2[:, :], yT[:, kc, tk], wt[:, kc, :], kc == 8, kc == 11, ['yT', wk_], ['ps_2'])
                              for kc in range(12, 16):
                                  p.mm(ps_3[:, :], yT[:, kc, tk], wt[:, kc, :], kc == 12, kc == 15, ['yT', wk_], ['ps_3'])
                              gk = 'gt%d' % a
                              p.tt(yv[:, :], ps_1[:, :], gt[a][:, 0, :], ALU.mult, ['ps_1', gk], ['yv'])
                              p.tt(tv[:, :], ps_2[:, :], gt[a][:, 1, :], ALU.mult, ['ps_2', gk], ['tv'])
                              p.tt(yv[:, :], yv[:, :], tv[:, :], ALU.add, ['yv', 'tv'], ['yv'], eng='pool')
                              p.tt(tv[:, :], ps_3[:, :], gt[a][:, 2, :], ALU.mult, ['ps_3', gk], ['tv'])
                              p.tt(ybm[j][:, cs], yv[:, :], tv[:, :], ALU.add, ['yv', 'tv'], ['ybm%d' % j], eng='pool')
                      for j in range(nt):
                          transpose_tile(ybm[j], 'ybm%d' % j, yT, 'yT', j * 128, 16, ps_t, ident)
                      for j in range(nt):
                          p.ts(rt[j][:, :], rt[j][:, :], float(ALPHA), None, ALU.mult, ALU.bypass, ['rt%d' % j], ['rt%d' % j], eng='pool')
                      oc = 0
                      for nch in range(4):
                          wt = wb[nch % 2]; wk_ = 'wb%d' % (nch % 2)
                          cs = slice(nch * 512, (nch + 1) * 512)
                          p.dmac(wt[:, :, :], T['w_out'][l * D:(l + 1) * D, cs].rearrange("(kc p) n -> p kc n", p=128), [], [wk_], wk_)
                          for j in range(nt):
                              ps = ps_o[oc % 2]; pk = 'ps_o%d' % (oc % 2); oc += 1
                              tk = slice(j * 128, (j + 1) * 128)
                              for kc in range(16):
                                  p.mm(ps[:, :], yT[:, kc, tk], wt[:, kc, :], kc == 0, kc == 15, ['yT', wk_], [pk])
                              p.tt(yv[:, :], ps[:, :], g1[r][:, cs], ALU.mult, [pk, 'g1' + ('c' if isc else 'l')], ['yv'])
                              p.tt(rt[j][:, cs], rt[j][:, cs], yv[:, :], ALU.add, ['rt%d' % j, 'yv'], ['rt%d' % j], eng='pool')
                      for j in range(nt):
                          rk = 'rt%d' % j
                          layer_norm_tile(sc, rt[j], rk, st, mv, rstd)
                          p.tt(rt[j][:, :], rt[j][:, :], lg[:, :], ALU.mult, [rk, 'lg'], [rk], eng='pool')
                          p.tt(rt[j][:, :], rt[j][:, :], lb[:, :], ALU.add, [rk, 'lb'], [rk], eng='pool')
                          p.dma(X[r0 + j * 128:r0 + (j + 1) * 128, :], rt[j][:, :], [rk], ['X'], 'o')
                  p.barrier()

              with Scope(nc) as sc:
                  ident = sc.sb("ident", [128, 128], BF16); identf = sc.sb("identf", [128, 128])
                  xt = [sc.sb("xt%d" % i, [128, D]) for i in range(2)]
                  hb = sc.sb("hb", [128, D], BF16)
                  st = sc.sb("st", [128, 4, 6]); mv = sc.sb("mv", [128, 2]); rstd = sc.sb("rstd", [128, 1])
                  hT = [sc.sb("hT%d" % i, [128, 16, 512], BF16) for i in range(2)]
                  tTf = sc.sb("tTf", [128, 16, 128])
                  wr = sc.sb("wr", [128, 16, 36])
                  rb = sc.sb("rb", [128, 36])
                  lgt = sc.sb("lgt", [128, 36]); gmx = sc.sb("gmx", [128, 1]); ge = sc.sb("ge", [128, 4]); gs = sc.sb("gs", [128, 1])
                  oh = sc.sb("oh", [128, 4]); em = sc.sb("em", [128, 4, 8]); m1 = sc.sb("m1", [128, 32]); m2 = sc.sb("m2", [128, 32])
                  t1v = sc.sb("t1v", [128, 1]); t2v = sc.sb("t2v", [128, 1]); w1v = sc.sb("w1v", [128, 1]); w2v = sc.sb("w2v", [128, 1])
                  G = [sc.sb("G%d" % i, [128, 32]) for i in range(2)]
                  ps_t = sc.ps("ps_t", [128, 4, 128], BF16)
                  ps_f = sc.ps("ps_f", [128, 4, 128])
                  ps_l = sc.ps("ps_l", [128, 36])
                  p.dma(identf[:, :], T['ident'][:, :], [], ['identf'], 'c')
                  p.cp(ident[:, :], identf[:, :], ['identf'], ['ident'])
                  p.dma(wr[:, :, 0:4], T['rg_w'][l * D:(l + 1) * D, :].rearrange("(kc p) n -> p kc n", p=128), [], ['wr'], 'c')
                  p.dma(wr[:, :, 4:36], T['re_w'][l * D:(l + 1) * D, :].rearrange("(kc p) n -> p kc n", p=128), [], ['wr'], 'c')
                  p.dma(rb[:, 0:4], T['rg_b'][l:l + 1, :].partition_broadcast(128), [], ['rb'], 'c')
                  p.dma(rb[:, 4:36], T['re_b'][l:l + 1, :].partition_broadcast(128), [], ['rb'], 'c')
                  scb = {}; shb = {}
                  for r, nm in ((0, 'l'), (1, 'c')):
                      if r == 1 and last:
                          continue
                      scb[r] = bc_load(sc, "scb" + nm, MOD[r:r + 1, 4 * D:5 * D])
                      shb[r] = bc_load(sc, "shb" + nm, MOD[r:r + 1, 3 * D:4 * D])
                  tcnt = 0
                  for gi, (r0, nt, isc) in enumerate(groups(not last)):
                      r = 1 if isc else 0
                      hT_ = hT[gi % 2]; hk = 'hT%d' % (gi % 2)
                      for j in range(nt):
                          x_ = xt[j % 2]; xk = 'xt%d' % (j % 2)
                          rows = slice(r0 + j * 128, r0 + (j + 1) * 128)
                          p.dma(x_[:, :], X[rows, :], ['X'], [xk], xk)
                          layer_norm_tile(sc, x_, xk, st, mv, rstd)
                          p.tt(x_[:, :], x_[:, :], scb[r][:, :], ALU.mult, [xk, 'scb' + ('c' if isc else 'l')], [xk], eng='pool')
                          p.tt(x_[:, :], x_[:, :], shb[r][:, :], ALU.add, [xk, 'shb' + ('c' if isc else 'l')], [xk], eng='pool')
                          p.cp(hb[:, :], x_[:, :], [xk], ['hb'], eng='act')
                          transpose_tile(hb, 'hb', hT_, hk, j * 128, 16, ps_t, ident)
                          for g in range(0, 16, 4):
                              for jj in range(4):
                                  p.tr(ps_f[:, jj, :], x_[:, (g + jj) * 128:(g + jj + 1) * 128], identf[:, :], [xk, 'identf'], ['ps_f'])
                              p.cp(tTf[:, g:g + 4, :], ps_f[:, :, :], ['ps_f'], ['tTf'], eng='act' if (g // 4) % 2 else 'dve')
                          for kc in range(16):
                              p.mm(ps_l[:, :], tTf[:, kc, :], wr[:, kc, :], kc == 0, kc == 15, ['tTf', 'wr'], ['ps_l'])
                          p.tt(lgt[:, :], ps_l[:, :], rb[:, :], ALU.add, ['ps_l', 'rb'], ['lgt'])
                          p.issue('dve', lambda e: e.reduce_max(gmx[:, :], lgt[:, 0:4], mybir.AxisListType.X), ['lgt'], ['gmx'])
                          p.ts(ge[:, :], lgt[:, 0:4], gmx[:, 0:1], None, ALU.subtract, ALU.bypass, ['lgt', 'gmx'], ['ge'])
                          p.ts(oh[:, :], ge[:, :], 0.0, None, ALU.is_ge, ALU.bypass, ['ge'], ['oh'])
                          p.act(ge[:, :], ge[:, :], AF.Exp, ['ge'], ['ge'])
                          p.issue('dve', lambda e: e.reduce_sum(gs[:, :], ge[:, :], mybir.AxisListType.X), ['ge'], ['gs'])
                          p.recip(gs[:, :], gs[:, :], ['gs'], ['gs'])
                          p.ts(oh[:, :], oh[:, :], 1.0, 1e30, ALU.subtract, ALU.mult, ['oh'], ['oh'])
                          for g4 in range(4):
                              p.ts(em[:, g4, :], lgt[:, 4 + 8 * g4:12 + 8 * g4], oh[:, g4:g4 + 1], None, ALU.add, ALU.bypass,
                                   ['lgt', 'oh'], ['em'])
                          emf = em[:, :, :].rearrange("p g e -> p (g e)")
                          p.issue('dve', lambda e, emf=emf: e.reduce_max(t1v[:, :], emf, mybir.AxisListType.X), ['em'], ['t1v'])
                          p.ts(m1[:, :], emf, t1v[:, 0:1], None, ALU.is_ge, ALU.bypass, ['em', 't1v'], ['m1'])
                          p.stt(m2[:, :], m1[:, :], -1e30, emf, ALU.mult, ALU.add, ['m1', 'em'], ['m2'])
                          p.issue('dve', lambda e: e.reduce_max(t2v[:, :], m2[:, :], mybir.AxisListType.X), ['m2'], ['t2v'])
                          p.ts(m2[:, :], m2[:, :], t2v[:, 0:1], None, ALU.is_ge, ALU.bypass, ['m2', 't2v'], ['m2'])
                          p.tt(t2v[:, :], t2v[:, :], t1v[:, :], ALU.subtract, ['t2v', 't1v'], ['t2v'])
                          p.act(t2v[:, :], t2v[:, :], AF.Exp, ['t2v'], ['t2v'])
                          p.ts(t2v[:, :], t2v[:, :], 1.0, None, ALU.add, ALU.bypass, ['t2v'], ['t2v'])
                          p.recip(w1v[:, :], t2v[:, :], ['t2v'], ['w1v'])
                          p.tt(w1v[:, :], w1v[:, :], gs[:, :], ALU.mult, ['w1v', 'gs'], ['w1v'])
                          p.tt(w2v[:, :], gs[:, :], w1v[:, :], ALU.subtract, ['gs', 'w1v'], ['w2v'])
                          G_ = G[tcnt % 2]; Gk = 'G%d' % (tcnt % 2); tcnt += 1
                          p.ts(G_[:, :], m1[:, :], w1v[:, 0:1], None, ALU.mult, ALU.bypass, ['m1', 'w1v'], [Gk])
                          p.stt(G_[:, :], m2[:, :], w2v[:, 0:1], G_[:, :], ALU.mult, ALU.add, ['m2', 'w2v', Gk], [Gk])
                          p.dma(GD[rows, :], G_[:, :], [Gk], ['GD'], 'o')
                      p.dma(TT[gi, :, :], hT_[:, :, :].rearrange("p k t -> p (k t)"), [hk], ['TT'], 'o')
                  p.barrier()

              with Scope(nc) as sc:
                  tT = [sc.sb("tT%d" % i, [128, 16, 512], BF16) for i in range(2)]
                  Gs = [sc.sb("Gs%d" % i, [128, 4, 32]) for i in range(2)]
                  acc = [sc.sb("acc%d" % i, [128, D], BF16) for i in range(8)]
                  w1s = [sc.sb("w1s%d" % i, [128, 16, 512], BF16) for i in range(2)]
                  w3s = [sc.sb("w3s%d" % i, [128, 16, 512], BF16) for i in range(2)]
                  w2s = [sc.sb("w2s%d" % i, [128, 4, D], BF16) for i in range(2)]
                  aT = [sc.sb("aT%d" % i, [128, 4, 512], BF16) for i in range(2)]
                  sl = [sc.sb("sl%d" % i, [128, 512]) for i in range(2)]
                  ps_1 = [sc.ps("ps_1%d" % i, [128, 512]) for i in range(2)]
                  ps_3 = [sc.ps("ps_3%d" % i, [128, 512]) for i in range(2)]
                  ps_o = [sc.ps("ps_o%d" % i, [128, 512]) for i in range(4)]
                  hcnt = 0; ocnt = 0; ecnt = 0; acnt = 0
                  glist = list(enumerate(groups(not last)))
                  for sg0 in range(0, len(glist), 2):
                      subs = glist[sg0:sg0 + 2]
                      for si, (gi, (r0, nt, isc)) in enumerate(subs):
                          p.dma(tT[si][:, :, :], TT[gi, :, :].rearrange("p (k t) -> p k t", k=16), ['TT'], ['tT%d' % si], 'tT%d' % si)
                          p.dma(Gs[si][:, 0:nt, :], GD[r0:r0 + nt * 128, :].rearrange("(j p) e -> p j e", p=128), ['GD'], ['Gs%d' % si], 'Gs%d' % si)
                      for e_ in range(NEXP):
                          a = ecnt % 2; ecnt += 1
                          wbase = (l * NEXP + e_) * D
                          p.dmac(w1s[a][:, :, :], T['moe_w1'][wbase:wbase + D, :].rearrange("(kc p) n -> p kc n", p=128), [], ['w1s%d' % a], 'w1s%d' % a)
                          p.dmac(w3s[a][:, :, :], T['moe_w3'][wbase:wbase + D, :].rearrange("(kc p) n -> p kc n", p=128), [], ['w3s%d' % a], 'w3s%d' % a)
                          w2base = (l * NEXP + e_) * EH
                          p.dmac(w2s[a][:, :, :], T['moe_w2'][w2base:w2base + EH, :].rearrange("(kc p) n -> p kc n", p=128), [], ['w2s%d' % a], 'w2s%d' % a)
                          for si, (gi, (r0, nt, isc)) in enumerate(subs):
                              ntok = nt * 128
                              tT_ = tT[si]; tk_ = 'tT%d' % si
                              G_ = Gs[si]; Gk = 'Gs%d' % si
                              ai = acnt % 2; acnt += 1
                              aT_ = aT[ai]; ak = 'aT%d' % ai
                              for hc in range(4):
                                  hb_ = hcnt % 2; hcnt += 1
                                  p1 = ps_1[hb_]; p3 = ps_3[hb_]
                                  for kc in range(16):
                                      p.mm(p1[:, 0:ntok], w1s[a][:, kc, hc * 128:(hc + 1) * 128], tT_[:, kc, 0:ntok], kc == 0, kc == 15,
                                           ['w1s%d' % a, tk_], ['ps_1%d' % hb_])
                                  for kc in range(16):
                                      p.mm(p3[:, 0:ntok], w3s[a][:, kc, hc * 128:(hc + 1) * 128], tT_[:, kc, 0:ntok], kc == 0, kc == 15,
                                           ['w3s%d' % a, tk_], ['ps_3%d' % hb_])
                                  s_ = sl[hb_]; sk_ = 'sl%d' % hb_
                                  p.act(s_[:, 0:ntok], p1[:, 0:ntok], AF.Silu, ['ps_1%d' % hb_], [sk_])
                                  p.tt(aT_[:, hc, 0:ntok], s_[:, 0:ntok], p3[:, 0:ntok], ALU.mult, [sk_, 'ps_3%d' % hb_], [ak + '_%d' % hc])
                              for j in range(nt):
                                  for dc in range(4):
                                      po = ps_o[ocnt % 4]; pk = 'ps_o%d' % (ocnt % 4); ocnt += 1
                                      for hc in range(4):
                                          p.mm(po[:, :], aT_[:, hc, j * 128:(j + 1) * 128], w2s[a][:, hc, dc * 512:(dc + 1) * 512], hc == 0, hc == 3,
                                               [ak + '_%d' % hc, 'w2s%d' % a], [pk])
                                      aj = si * 4 + j
                                      dst = acc[aj][:, dc * 512:(dc + 1) * 512]
                                      if e_ == 0:
                                          p.ts(dst, po[:, :], G_[:, j, e_:e_ + 1], None, ALU.mult, ALU.bypass, [pk, Gk], ['acc%d_%d' % (aj, dc)])
                                      else:
                                          p.stt(dst, po[:, :], G_[:, j, e_:e_ + 1], dst, ALU.mult, ALU.add, [pk, Gk, 'acc%d_%d' % (aj, dc)],
                                                ['acc%d_%d' % (aj, dc)])
                      for si, (gi, (r0, nt, isc)) in enumerate(subs):
                          for j in range(nt):
                              aj = si * 4 + j
                              p.dma(MD[r0 + j * 128:r0 + (j + 1) * 128, :], acc[aj][:, :], ['acc%d_%d' % (aj, dc) for dc in range(4)], ['MD'], 'o')
                  p.barrier()

              with Scope(nc) as sc:
                  xt = [sc.sb("xt%d" % i, [128, D]) for i in range(2)]
                  mt = [sc.sb("mt%d" % i, [128, D], BF16) for i in range(2)]
                  m32 = sc.sb("m32", [128, D])
                  st = sc.sb("st", [128, 4, 6]); mv = sc.sb("mv", [128, 2]); rstd = sc.sb("rstd", [128, 1])
                  lg = bc_load(sc, "lg", T['ln2_g'][l:l + 1, :]); lb = bc_load(sc, "lb", T['ln2_b'][l:l + 1, :])
                  g2 = {0: bc_load(sc, "g2l", MOD[0:1, 5 * D:6 * D])}
                  if not last:
                      g2[1] = bc_load(sc, "g2c", MOD[1:2, 5 * D:6 * D])
                  it = 0
                  for (r0, nt, isc) in groups(not last):
                      r = 1 if isc else 0
                      for j in range(nt):
                          a = it % 2; it += 1
                          rows = slice(r0 + j * 128, r0 + (j + 1) * 128)
                          xk = 'xt%d' % a; mk_ = 'mt%d' % a
                          p.dma(xt[a][:, :], X[rows, :], ['X'], [xk], xk)
                          p.dma(mt[a][:, :], MD[rows, :], ['MD'], [mk_], mk_)
                          p.tt(m32[:, :], mt[a][:, :], g2[r][:, :], ALU.mult, [mk_, 'g2' + ('c' if isc else 'l')], ['m32'], eng='pool')
                          p.stt(xt[a][:, :], xt[a][:, :], float(ALPHA), m32[:, :], ALU.mult, ALU.add, [xk, 'm32'], [xk])
                          layer_norm_tile(sc, xt[a], xk, st, mv, rstd)
                          p.tt(xt[a][:, :], xt[a][:, :], lg[:, :], ALU.mult, [xk, 'lg'], [xk], eng='pool')
                          p.tt(xt[a][:, :], xt[a][:, :], lb[:, :], ALU.add, [xk, 'lb'], [xk], eng='pool')
                          if l == depth - 1 and not isc:
                              orow = b * SEQ + r0 + j * 128
                              p.dma(OUT[orow:orow + 128, :], xt[a][:, :], [xk], ['OUT'], 'o')
                          else:
                              p.dma(X[rows, :], xt[a][:, :], [xk], ['X'], 'o')
                  p.barrier()
    except _Stop:
        pass
    p.barrier = _orig_barrier
    p.finish()
    return nc, p


def _flat(a, shape):
    return np.ascontiguousarray(np.asarray(a, dtype=np.float32).reshape(shape))


def make_in_maps(inputs, NC, NB):
    cs = _consts()
    shared = {n: _flat(inputs[n], s) for n, s in WEIGHT_SHAPES.items()}
    shared.update({n: cs[n] for n in CONST_SHAPES})
    shared['c_ctx'] = _flat(inputs['c_ctx'], [16, 128])
    maps = []
    for c in range(NC):
        m = dict(shared)
        bs = slice(c * NB, (c + 1) * NB)
        m['x'] = _flat(np.asarray(inputs['x'])[bs], [NB * SEQ, D])
        m['ctx'] = _flat(np.asarray(inputs['ctx'])[bs], [NB * CTX, D])
        m['c'] = _flat(np.asarray(inputs['c'])[bs], [NB * 16, 128])
        maps.append(m)
    return maps


def kernel(**inputs):
    NC = NC_ACTIVE
    NB = 4 // NC
    nc, _ = build_nc(NB)
    maps = make_in_maps(inputs, NC, NB)
    res = run_bass_kernel_spmd(nc, maps, core_ids=list(range(NC)))
    out = np.concatenate([r["out"].reshape(NB, SEQ, D) for r in res.results], axis=0)
    return out.astype(np.float32)
```
